# Optimizing a Trainium2 kernel written in Bass

```python
import jax
import jax.numpy as jnp
from jax import lax
import numpy as np

D_MODEL = 2048
BATCH = 8
SEQ = 4096
DEPTH = 2

D_MIX = D_MODEL
N_GROUPS = 4
GROUP_W = D_MIX // N_GROUPS
HG_HEADS = 4
HG_DIM = GROUP_W // HG_HEADS
HG_CHUNK = 64
RW_HEAD = 64
RW_HEADS = GROUP_W // RW_HEAD
RW_W_LORA = 32
RW_A_LORA = 32
RW_G_LORA = 96
RW_LN_EPS = 64e-5
ML_HEADS = 4
ML_DIM = GROUP_W // ML_HEADS
ML_CHUNK = 64
ML_CONV = 4
ML_QK_BLOCK = 4
NEG_BIG = -1e30
LRU_BLOCKS = 4
LRU_BLOCK = GROUP_W // LRU_BLOCKS
LRU_C = 8.0
LRU_CONV = 4
D_FF = 5632
N_SUB = 3
NORM_EPS = 1e-6
HG_IN = 4 * GROUP_W
RW_IN = 3 * GROUP_W + RW_W_LORA + RW_A_LORA + RW_G_LORA
ML_IN = 3 * GROUP_W + 2 * ML_HEADS
LRU_IN = 2 * GROUP_W
D_IN = HG_IN + RW_IN + ML_IN + LRU_IN

kernel_name = "hymba_style_hgrn2_rwkv7_mlstm_rglru_macaron_adaln"


def _split(a, sizes):
    return jnp.split(a, np.cumsum(sizes)[:-1].tolist(), axis=-1)


def _rmsnorm(x, w, eps=NORM_EPS):
    x32 = x.astype(jnp.float32)
    y = x32 * lax.rsqrt(jnp.mean(x32 * x32, axis=-1, keepdims=True) + eps)
    return (y * w.astype(jnp.float32)).astype(x.dtype)


def _head_rmsnorm(x, w, n_heads, eps=NORM_EPS):
    shp = x.shape
    xh = x.astype(jnp.float32).reshape(*shp[:-1], n_heads, shp[-1] // n_heads)
    xh = xh * lax.rsqrt(jnp.mean(xh * xh, axis=-1, keepdims=True) + eps)
    return xh.reshape(shp) * w.astype(jnp.float32)


def _modulate(x, gain, shift, scale):
    return _rmsnorm(x, gain) * (1.0 + scale[:, None, :]) + shift[:, None, :]


def _swiglu(h, w1, w3, w2):
    return (jax.nn.silu(h @ w1) * (h @ w3)) @ w2


def _token_shift(u):
    return jnp.pad(u[:, :-1], ((0, 0), (1, 0), (0, 0)))


def _causal_dwconv(x, w, b):
    width = w.shape[0]
    y = lax.conv_general_dilated(
        x, w[:, None, :].astype(x.dtype), window_strides=(1,), padding=[(width - 1, 0)],
        dimension_numbers=("NWC", "WIO", "NWC"), feature_group_count=x.shape[-1])
    return y + b


def _to_chunks(a, chunk):
    bsz, seqlen = a.shape[:2]
    a = a.reshape(bsz, seqlen // chunk, chunk, *a.shape[2:])
    return a.transpose((1, 0, 3, 2) + tuple(range(4, a.ndim)))


def _from_chunks(a):
    nc, bsz, nh, chunk = a.shape[:4]
    a = a.transpose((1, 0, 3, 2) + tuple(range(4, a.ndim)))
    return a.reshape(bsz, nc * chunk, nh, *a.shape[4:])


def _gla_chunked(q, k, v, log_f):
    bsz, _, nh, dk = q.shape
    dv = v.shape[-1]
    causal = jnp.tril(jnp.ones((HG_CHUNK, HG_CHUNK), bool))[:, :, None]

    def step(S, inp):
        q_c, k_c, v_c, g_c = inp
        b = jnp.cumsum(g_c, axis=2)
        diff = b[:, :, :, None, :] - b[:, :, None, :, :]
        decay = jnp.where(causal, jnp.exp(jnp.where(causal, diff, 0.0)), 0.0)
        scores = jnp.einsum("bhtk,bhtsk,bhsk->bhts", q_c, decay, k_c)
        o = (jnp.einsum("bhts,bhsv->bhtv", scores, v_c)
             + jnp.einsum("bhtk,bhkv->bhtv", q_c * jnp.exp(b), S))
        b_last = b[:, :, -1:, :]
        S = (jnp.exp(b_last[:, :, 0, :])[..., None] * S
             + jnp.einsum("bhsk,bhsv->bhkv", k_c * jnp.exp(b_last - b), v_c))
        return S, o

    S0 = jnp.zeros((bsz, nh, dk, dv), jnp.float32)
    _, o = lax.scan(step, S0, tuple(_to_chunks(a, HG_CHUNK) for a in (q, k, v, log_f)))
    return _from_chunks(o)


def _hgrn2(u, lb, g_norm):
    bsz, seqlen, _ = u.shape
    q_raw, f_raw, i_raw, g_raw = _split(u, (GROUP_W,) * 4)
    heads = lambda a: a.reshape(bsz, seqlen, HG_HEADS, HG_DIM)
    q = jax.nn.silu(q_raw)
    log_f = jnp.log(lb + (1.0 - lb) * jax.nn.sigmoid(f_raw))
    k = (1.0 - lb) * jax.nn.sigmoid(-f_raw)
    o = _gla_chunked(heads(q), heads(k), heads(i_raw), heads(log_f))
    o = _head_rmsnorm(o.reshape(bsz, seqlen, GROUP_W), g_norm, HG_HEADS)
    return o * jax.nn.silu(g_raw)


def _wkv7_scan(r, w, k, v, a, b):
    bsz, _, nh, n = r.shape

    def step(S, inp):
        r_t, w_t, k_t, v_t, a_t, b_t = inp
        sa = jnp.einsum("bhvk,bhk->bhv", S, a_t)
        S = (S * w_t[:, :, None, :] + sa[..., None] * b_t[:, :, None, :]
             + v_t[..., None] * k_t[:, :, None, :])
        return S, jnp.einsum("bhvk,bhk->bhv", S, r_t)

    S0 = jnp.zeros((bsz, nh, n, n), jnp.float32)
    _, o = lax.scan(step, S0, tuple(jnp.moveaxis(t, 1, 0) for t in (r, w, k, v, a, b)))
    return jnp.moveaxis(o, 0, 1)


def _rwkv7(u, mu, w0, w2, a0, a2, g2, k_k, k_a, r_k, ln_w, ln_b):
    bsz, seqlen, _ = u.shape
    u = u + (_token_shift(u) - u) * mu
    r, k, v, wd, ad, gd = _split(u, (GROUP_W, GROUP_W, GROUP_W, RW_W_LORA, RW_A_LORA, RW_G_LORA))
    w_log = -jax.nn.softplus(-(w0 + jnp.tanh(wd) @ w2)) - 0.5
    decay = jnp.exp(-jnp.exp(w_log))
    a = jax.nn.sigmoid(a0 + ad @ a2)
    g = jax.nn.sigmoid(gd) @ g2
    heads = lambda t: t.reshape(bsz, seqlen, RW_HEADS, RW_HEAD)
    kk = heads(k * k_k)
    kk = kk / jnp.maximum(jnp.sqrt(jnp.sum(kk * kk, axis=-1, keepdims=True) + 1e-12), 1e-6)
    k = k * (1.0 + (a - 1.0) * k_a)
    rh, kh, vh = heads(r), heads(k), heads(v)
    o = _wkv7_scan(rh, heads(decay), kh, vh, -kk, kk * heads(a))
    mean = jnp.mean(o, axis=-1, keepdims=True)
    var = jnp.mean(jnp.square(o - mean), axis=-1, keepdims=True)
    o = ((o - mean) * lax.rsqrt(var + RW_LN_EPS)).reshape(bsz, seqlen, GROUP_W) * ln_w + ln_b
    bonus = jnp.sum(rh * kh * r_k, axis=-1, keepdims=True) * vh
    return (o + bonus.reshape(bsz, seqlen, GROUP_W)) * g


def _mlstm_chunked(q, k, v, i_pre, log_f):
    bsz, _, nh, dk = q.shape
    dv = v.shape[-1]
    causal = jnp.tril(jnp.ones((ML_CHUNK, ML_CHUNK), bool))

    def step(carry, inp):
        M, n, m = carry
        q_c, k_c, v_c, i_c, f_c = inp
        b = jnp.cumsum(f_c, axis=-1)
        lw = b[..., :, None] - b[..., None, :] + i_c[..., None, :]
        lprev = b + m[..., None]
        m_t = jnp.maximum(lprev, jnp.max(jnp.where(causal, lw, NEG_BIG), axis=-1))
        wts = jnp.where(causal, jnp.exp(jnp.where(causal, lw - m_t[..., None], 0.0)), 0.0)
        s = jnp.einsum("bhtd,bhsd->bhts", q_c, k_c) * wts
        wp = jnp.exp(lprev - m_t)
        num = jnp.einsum("bhts,bhsv->bhtv", s, v_c) + wp[..., None] * jnp.einsum("bhtk,bhkv->bhtv", q_c, M)
        den = jnp.sum(s, axis=-1) + wp * jnp.einsum("bhtk,bhk->bht", q_c, n)
        h = num / jnp.maximum(jnp.abs(den), jnp.exp(-m_t))[..., None]
        m_new = m_t[..., -1]
        wl = jnp.exp(b[..., -1:] - b + i_c - m_new[..., None])
        dec = jnp.exp(b[..., -1] + m - m_new)
        M = dec[..., None, None] * M + jnp.einsum("bhs,bhsk,bhsv->bhkv", wl, k_c, v_c)
        n = dec[..., None] * n + jnp.einsum("bhs,bhsk->bhk", wl, k_c)
        return (M, n, m_new), h

    carry0 = (jnp.zeros((bsz, nh, dk, dv), jnp.float32), jnp.zeros((bsz, nh, dk), jnp.float32),
              jnp.zeros((bsz, nh), jnp.float32))
    _, h = lax.scan(step, carry0, tuple(_to_chunks(a, ML_CHUNK) for a in (q, k, v, i_pre, log_f)))
    return _from_chunks(h)


def _mlstm(u, conv_w, conv_b, wq, wk, i_b, f_b, norm_w, skip):
    bsz, seqlen, _ = u.shape
    xm, v, o_raw, i_raw, f_raw = _split(u, (GROUP_W, GROUP_W, GROUP_W, ML_HEADS, ML_HEADS))
    xc = jax.nn.silu(_causal_dwconv(xm, conv_w, conv_b))
    blocks = xc.reshape(bsz, seqlen, GROUP_W // ML_QK_BLOCK, ML_QK_BLOCK)
    q = jnp.einsum("btnj,nji->btni", blocks, wq).reshape(bsz, seqlen, ML_HEADS, ML_DIM) * ML_DIM ** -0.5
    k = jnp.einsum("btnj,nji->btni", blocks, wk).reshape(bsz, seqlen, ML_HEADS, ML_DIM)
    h = _mlstm_chunked(q, k, v.reshape(bsz, seqlen, ML_HEADS, ML_DIM),
                       i_raw + i_b, jax.nn.log_sigmoid(f_raw + f_b))
    h = jax.nn.sigmoid(o_raw) * h.reshape(bsz, seqlen, GROUP_W)
    return _head_rmsnorm(h, norm_w, ML_HEADS) + skip * xc


def _rglru(u, conv_w, conv_b, wa, ba, wx, bx, lam, norm_w):
    bsz, seqlen, _ = u.shape
    xb, gb = _split(u, (GROUP_W, GROUP_W))
    xc = _causal_dwconv(xb, conv_w, conv_b)
    blocks = xc.reshape(bsz, seqlen, LRU_BLOCKS, LRU_BLOCK)
    r = jax.nn.sigmoid(jnp.einsum("btni,nij->btnj", blocks, wa).reshape(bsz, seqlen, GROUP_W) + ba)
    i = jax.nn.sigmoid(jnp.einsum("btni,nij->btnj", blocks, wx).reshape(bsz, seqlen, GROUP_W) + bx)
    log_a = -LRU_C * r * jax.nn.softplus(-lam)
    a = jnp.exp(log_a)
    bterm = jnp.sqrt(-jnp.expm1(2.0 * log_a)) * (i * xc)

    def combine(p, s):
        return p[0] * s[0], s[0] * p[1] + s[1]

    _, h = lax.associative_scan(combine, (a, bterm), axis=1)
    return _rmsnorm(h * jax.nn.gelu(gb), norm_w)


def setup_inputs(seed: int = 0) -> dict:
    key = jax.random.key(seed)
    ks = iter(jax.random.split(key, 64))

    def nrm(shape, scale):
        return scale * jax.random.normal(next(ks), shape, jnp.float32)

    chan = jnp.arange(GROUP_W, dtype=jnp.float32) / (GROUP_W - 1)
    a8 = jax.random.uniform(next(ks), (DEPTH, GROUP_W), jnp.float32, 0.9, 0.999)
    a_lru = a8 ** (1.0 / LRU_C)
    return {
        "x": nrm((BATCH, SEQ, D_MODEL), 1.0),
        "c": nrm((BATCH, D_MODEL), 1.0),
        "norm_gain": 1.0 + nrm((DEPTH, N_SUB, D_MODEL), 0.02),
        "mod_w": nrm((DEPTH, D_MODEL, N_SUB * 3 * D_MODEL), 0.2 * D_MODEL ** -0.5),
        "mod_b": nrm((DEPTH, N_SUB * 3 * D_MODEL), 0.02),
        "ffn_w1": nrm((DEPTH, 2, D_MODEL, D_FF), D_MODEL ** -0.5),
        "ffn_w3": nrm((DEPTH, 2, D_MODEL, D_FF), D_MODEL ** -0.5),
        "ffn_w2": nrm((DEPTH, 2, D_FF, D_MODEL), D_FF ** -0.5),
        "w_in": nrm((DEPTH, D_MODEL, D_IN), D_MODEL ** -0.5),
        "w_out": nrm((DEPTH, D_MIX, D_MODEL), D_MIX ** -0.5),
        "hg_lb_logits": nrm((DEPTH, GROUP_W), 0.5),
        "hg_norm": 1.0 + nrm((DEPTH, GROUP_W), 0.02),
        "rw_mu": jax.random.uniform(next(ks), (DEPTH, RW_IN), jnp.float32, 0.0, 1.0),
        "rw_w0": (-6.5 + 5.0 * chan ** 0.85) + nrm((DEPTH, GROUP_W), 0.1),
        "rw_w2": nrm((DEPTH, RW_W_LORA, GROUP_W), 0.1),
        "rw_a0": nrm((DEPTH, GROUP_W), 0.1),
        "rw_a2": nrm((DEPTH, RW_A_LORA, GROUP_W), 0.1),
        "rw_g2": nrm((DEPTH, RW_G_LORA, GROUP_W), RW_G_LORA ** -0.5),
        "rw_kk": 0.85 + nrm((DEPTH, GROUP_W), 0.02),
        "rw_ka": 1.0 + nrm((DEPTH, GROUP_W), 0.02),
        "rw_rk": -0.04 + nrm((DEPTH, RW_HEADS, RW_HEAD), 0.02),
        "rw_ln_w": 1.0 + nrm((DEPTH, GROUP_W), 0.02),
        "rw_ln_b": nrm((DEPTH, GROUP_W), 0.02),
        "ml_conv_w": nrm((DEPTH, ML_CONV, GROUP_W), ML_CONV ** -0.5),
        "ml_conv_b": nrm((DEPTH, GROUP_W), 0.02),
        "ml_wq": nrm((DEPTH, GROUP_W // ML_QK_BLOCK, ML_QK_BLOCK, ML_QK_BLOCK), ML_QK_BLOCK ** -0.5),
        "ml_wk": nrm((DEPTH, GROUP_W // ML_QK_BLOCK, ML_QK_BLOCK, ML_QK_BLOCK), ML_QK_BLOCK ** -0.5),
        "ml_i_b": nrm((DEPTH, ML_HEADS), 0.1),
        "ml_f_b": jnp.linspace(3.0, 6.0, ML_HEADS, dtype=jnp.float32) + nrm((DEPTH, ML_HEADS), 0.1),
        "ml_norm": 1.0 + nrm((DEPTH, GROUP_W), 0.02),
        "ml_skip": 1.0 + nrm((DEPTH, GROUP_W), 0.02),
        "lru_conv_w": nrm((DEPTH, LRU_CONV, GROUP_W), LRU_CONV ** -0.5),
        "lru_conv_b": nrm((DEPTH, GROUP_W), 0.02),
        "lru_wa": nrm((DEPTH, LRU_BLOCKS, LRU_BLOCK, LRU_BLOCK), LRU_BLOCK ** -0.5),
        "lru_ba": nrm((DEPTH, GROUP_W), 0.02),
        "lru_wx": nrm((DEPTH, LRU_BLOCKS, LRU_BLOCK, LRU_BLOCK), LRU_BLOCK ** -0.5),
        "lru_bx": nrm((DEPTH, GROUP_W), 0.02),
        "lru_lambda": jnp.log(a_lru) - jnp.log1p(-a_lru),
        "lru_norm": 1.0 + nrm((DEPTH, GROUP_W), 0.02),
        "final_norm": 1.0 + nrm((D_MODEL,), 0.02),
    }


def reference(x, c, norm_gain, mod_w, mod_b, ffn_w1, ffn_w3, ffn_w2, w_in, w_out,
              hg_lb_logits, hg_norm, rw_mu, rw_w0, rw_w2, rw_a0, rw_a2, rw_g2, rw_kk, rw_ka,
              rw_rk, rw_ln_w, rw_ln_b, ml_conv_w, ml_conv_b, ml_wq, ml_wk, ml_i_b, ml_f_b,
              ml_norm, ml_skip, lru_conv_w, lru_conv_b, lru_wa, lru_ba, lru_wx, lru_bx,
              lru_lambda, lru_norm, final_norm):
    lb_w = jax.nn.softmax(hg_lb_logits.astype(jnp.float32), axis=0)
    lower_bounds = jnp.cumsum(lb_w, axis=0) - lb_w[0]
    c_act = jax.nn.silu(c)
    for l in range(DEPTH):
        mod = (c_act @ mod_w[l] + mod_b[l]).reshape(c.shape[0], N_SUB, 3, D_MODEL)
        shift, scale, gate = mod[:, :, 0], mod[:, :, 1], mod[:, :, 2]

        h = _modulate(x, norm_gain[l, 0], shift[:, 0], scale[:, 0])
        x = x + 0.5 * (1.0 + gate[:, 0, None, :]) * _swiglu(h, ffn_w1[l, 0], ffn_w3[l, 0], ffn_w2[l, 0])

        h = _modulate(x, norm_gain[l, 1], shift[:, 1], scale[:, 1])
        u = (h @ w_in[l]).astype(jnp.float32)
        u_hg, u_rw, u_ml, u_lru = _split(u, (HG_IN, RW_IN, ML_IN, LRU_IN))
        y_hg = _hgrn2(u_hg, lower_bounds[l], hg_norm[l])
        y_rw = _rwkv7(u_rw, rw_mu[l], rw_w0[l], rw_w2[l], rw_a0[l], rw_a2[l], rw_g2[l],
                      rw_kk[l], rw_ka[l], rw_rk[l], rw_ln_w[l], rw_ln_b[l])
        y_ml = _mlstm(u_ml, ml_conv_w[l], ml_conv_b[l], ml_wq[l], ml_wk[l], ml_i_b[l],
                      ml_f_b[l], ml_norm[l], ml_skip[l])
        y_lru = _rglru(u_lru, lru_conv_w[l], lru_conv_b[l], lru_wa[l], lru_ba[l], lru_wx[l],
                       lru_bx[l], lru_lambda[l], lru_norm[l])
        y = jnp.concatenate([y_hg, y_rw, y_ml, y_lru], axis=-1).astype(x.dtype)
        x = x + (1.0 + gate[:, 1, None, :]) * (y @ w_out[l])

        h = _modulate(x, norm_gain[l, 2], shift[:, 2], scale[:, 2])
        x = x + 0.5 * (1.0 + gate[:, 2, None, :]) * _swiglu(h, ffn_w1[l, 1], ffn_w3[l, 1], ffn_w2[l, 1])
    return _rmsnorm(x, final_norm)
```

```python
import numpy as np
import concourse.bass as bass
import concourse.mybir as mybir
from concourse.bass_utils import run_bass_kernel_spmd
from contextlib import ExitStack

F32 = mybir.dt.float32
BF16 = mybir.dt.bfloat16
AF = mybir.ActivationFunctionType
ALU = mybir.AluOpType

D = 2048
DFF = 5632
DIN = 6312
TT = 512
NFC = 16
C_HG, C_RW, C_ML, C_LRU = 0, 2048, 3744, 5288
EXPM05 = 0.6065306597126334
import os as _os
STRICT = bool(int(_os.environ.get('K_STRICT', '0')))


class V:
    __slots__ = ("ap", "tid", "lo", "hi")

    def __init__(s, ap, tid, lo, hi):
        s.ap, s.tid, s.lo, s.hi = ap, tid, lo, hi

    def m(s, f):
        return V(f(s.ap), s.tid, s.lo, s.hi)


class Buf:
    def __init__(s, ap, tid, shape, esz, base=0, dram=False):
        s.ap, s.tid, s.shape, s.esz, s.base, s.dram = ap, tid, tuple(shape), esz, base, dram
        dims = s.shape if dram else s.shape[1:]
        st = []
        acc = esz
        for d in reversed(dims):
            st.append(acc)
            acc *= d
        s.strides = list(reversed(st))
        s.nbytes = acc

    def __getitem__(s, idx):
        if not isinstance(idx, tuple):
            idx = (idx,)
        fidx = idx if s.dram else idx[1:]
        dims = s.shape if s.dram else s.shape[1:]
        lo = s.base
        hi = s.base
        for i, d in enumerate(dims):
            if i < len(fidx):
                k = fidx[i]
                if isinstance(k, int):
                    a, b = k, k + 1
                else:
                    a = 0 if k.start is None else k.start
                    b = d if k.stop is None else k.stop
            else:
                a, b = 0, d
            assert 0 <= a < b <= d, (s.tid, idx, s.shape)
            lo += a * s.strides[i]
            hi += (b - 1) * s.strides[i]
        hi += s.esz
        if s.tid.startswith("ps") and not s.dram and len(s.tid) == 3:
            lo, hi = s.base, s.base + s.nbytes
        return V(s.ap[idx], s.tid, lo, hi)

    def all(s):
        return V(s.ap, s.tid, s.base, s.base + s.nbytes)


class Sched:
    ENG = ("pe", "act", "dve", "pool", "sp")

    def __init__(s, nc, es, ndma=40):
        s.nc = nc
        s.ins = {e: [] for e in s.ENG}
        s.known = {e: {} for e in s.ENG}
        s.recs = {}
        s.sem = {e: es.enter_context(nc.semaphore("s_" + e)) for e in s.ENG}
        s.ndma = ndma
        s.dsem = [es.enter_context(nc.semaphore("d%d" % i)) for i in range(ndma)]
        s.dtarget = [0] * ndma
        s.drr = 0
        s.drr_sw = 0

    def _deps(s, eng, r, w):
        deps = {}

        def add(rec, is_read_dep):
            key, idx = rec[3], rec[4]
            if key == eng:
                if eng == "pe":
                    return
                if not STRICT and not (is_read_dep and rec[2] == "w"):
                    return
            if deps.get(key, 0) < idx:
                deps[key] = idx

        for v in r:
            for rec in s.recs.get(v.tid, ()):
                if rec[2] == "w" and rec[0] < v.hi and v.lo < rec[1]:
                    add(rec, True)
        for v in w:
            for rec in s.recs.get(v.tid, ()):
                if rec[0] < v.hi and v.lo < rec[1]:
                    add(rec, False)
        return deps

    def _commit(s, eng, deps, r, w, key, idx):
        waits = []
        kn = s.known[eng]
        for k, i in deps.items():
            if kn.get(k, 0) < i:
                kn[k] = i
                waits.append((k, i))
                if not isinstance(k, tuple):
                    s.ins[k][i - 1]["inc"] = True
        for v in w:
            lst = s.recs.setdefault(v.tid, [])
            lst[:] = [rc for rc in lst if not (rc[0] >= v.lo and rc[1] <= v.hi)]
            lst.append([v.lo, v.hi, "w", key, idx])
        for v in r:
            lst = s.recs.setdefault(v.tid, [])
            for rc in lst:
                if rc[2] == "r" and rc[3] == key and rc[0] == v.lo and rc[1] == v.hi:
                    rc[4] = idx
                    break
            else:
                lst.append([v.lo, v.hi, "r", key, idx])
        return waits

    def op(s, eng, fn, r, w):
        deps = s._deps(eng, r, w)
        idx = len(s.ins[eng]) + 1
        waits = s._commit(eng, deps, r, w, eng, idx)
        s.ins[eng].append({"fn": fn, "waits": waits, "inc": False, "dma": None})

    def dma(s, q, out, in_):
        if q == "pool":
            slot = s.ndma - 8 + s.drr_sw
            s.drr_sw = (s.drr_sw + 1) % 8
        else:
            slot = s.drr
            s.drr = (s.drr + 1) % (s.ndma - 8)
        deps = s._deps(q, [in_], [out])
        if s.dtarget[slot] > 0:
            deps[("d", slot)] = s.dtarget[slot]
        s.dtarget[slot] += 16
        key, idx = ("d", slot), s.dtarget[slot]
        waits = s._commit(q, deps, [in_], [out], key, idx)
        s.ins[q].append({"fn": (lambda e, o=out.ap, i=in_.ap: e.dma_start(out=o, in_=i)),
                         "waits": waits, "inc": False, "dma": slot})

    def emit(s, block):
        pref = {}
        for e in s.ENG:
            c = 0
            arr = []
            for it in s.ins[e]:
                if it["inc"]:
                    c += 1
                arr.append(c)
            pref[e] = arr

        def run(e, name):
            for it in s.ins[name]:
                for k, i in it["waits"]:
                    if isinstance(k, tuple):
                        e.wait_ge(s.dsem[k[1]], i)
                    else:
                        e.wait_ge(s.sem[k], pref[k][i - 1])
                inst = it["fn"](e)
                if it["dma"] is not None:
                    inst.then_inc(s.dsem[it["dma"]], 16)
                elif it["inc"]:
                    inst.then_inc(s.sem[name], 1)
            if name == "sp":
                for sl in range(s.ndma):
                    if s.dtarget[sl] > 0:
                        e.wait_ge(s.dsem[sl], s.dtarget[sl])
                for k in ("pe", "act", "dve", "pool"):
                    if pref[k] and pref[k][-1] > 0:
                        e.wait_ge(s.sem[k], pref[k][-1])

        block.tensor(lambda e: run(e, "pe"))
        block.scalar(lambda e: run(e, "act"))
        block.vector(lambda e: run(e, "dve"))
        block.gpsimd(lambda e: run(e, "pool"))
        block.sync(lambda e: run(e, "sp"))

    @staticmethod
    def _sc(x):
        return (x.ap, [x]) if isinstance(x, V) else (x, [])

    def tt(s, out, a, b, op, eng="dve"):
        s.op(eng, lambda e: e.tensor_tensor(out=out.ap, in0=a.ap, in1=b.ap, op=op), [a, b], [out])

    def ts(s, out, a, s1, op0, s2=None, op1=None, eng="dve"):
        a1, r1 = s._sc(s1)
        a2, r2 = s._sc(s2) if s2 is not None else (None, [])
        if op1 is None:
            s.op(eng, lambda e: e.tensor_scalar(out=out.ap, in0=a.ap, scalar1=a1, scalar2=None, op0=op0),
                 [a] + r1, [out])
        else:
            s.op(eng, lambda e: e.tensor_scalar(out=out.ap, in0=a.ap, scalar1=a1, scalar2=a2, op0=op0, op1=op1),
                 [a] + r1 + r2, [out])

    def stt(s, out, a, sc, b, op0, op1):
        a1, r1 = s._sc(sc)
        s.op("dve", lambda e: e.scalar_tensor_tensor(out=out.ap, in0=a.ap, scalar=a1, in1=b.ap, op0=op0, op1=op1),
             [a, b] + r1, [out])

    def act(s, out, a, func, bias=None, scale=1.0):
        ab, rb = s._sc(bias) if bias is not None else (None, [])
        asc, rs = s._sc(scale)
        if ab is None:
            s.op("act", lambda e: e.activation(out=out.ap, in_=a.ap, func=func, scale=asc), [a] + rs, [out])
        else:
            s.op("act", lambda e: e.activation(out=out.ap, in_=a.ap, func=func, bias=ab, scale=asc),
                 [a] + rb + rs, [out])

    def cp(s, out, a, eng="dve"):
        if eng == "act":
            s.act(out, a, AF.Copy)
        else:
            s.op(eng, lambda e: e.tensor_copy(out=out.ap, in_=a.ap), [a], [out])

    def _pemode(s, sig):
        if getattr(s, "_pesig", None) not in (None, sig):
            s.op("pe", lambda e: e.drain(), [], [])
        s._pesig = sig

    def mm(s, out, lhsT, rhs, start=True, stop=True):
        shp = lhsT.ap.shape
        rnd = lambda n: 32 if n <= 32 else (64 if n <= 64 else 128)
        s._pemode((rnd(shp[0]), rnd(int(np.prod(shp[1:])))))
        s.op("pe", lambda e: e.matmul(out.ap, lhsT=lhsT.ap, rhs=rhs.ap, start=start, stop=stop), [lhsT, rhs], [out])

    def tr(s, out, a, ident):
        s._pemode((128, 128))
        s.op("pe", lambda e: e.transpose(out.ap, a.ap, ident.ap), [a, ident], [out])

    def scan(s, out, d0, d1, init, op0, op1):
        ai, ri = s._sc(init)
        s.op("dve", lambda e: e.tensor_tensor_scan(out=out.ap, data0=d0.ap, data1=d1.ap, initial=ai, op0=op0, op1=op1),
             [d0, d1] + ri, [out])

    def recip(s, out, a):
        s.op("dve", lambda e: e.reciprocal(out=out.ap, in_=a.ap), [a], [out])

    def memset(s, out, val, eng="dve"):
        s.op(eng, lambda e: e.memset(out.ap, val), [], [out])


def _pv_layout(depth):
    names = []
    for l in range(depth):
        for sub in range(3):
            names.append(("gain%d%d" % (l, sub), 16))
        names.append(("modb%d" % l, 144))
        for n, k in (("hglog", 4), ("hgnorm", 4), ("mur", 4), ("muk", 4), ("muv", 4), ("w0", 4), ("a0", 4),
                     ("kk", 4), ("ka", 4), ("rk", 4), ("lnw", 4), ("lnb", 4), ("mlcw", 16), ("mlcb", 4),
                     ("mlwq", 16), ("mlwk", 16), ("mlnorm", 4), ("mlskip", 4), ("lrucw", 16), ("lrucb", 4),
                     ("lruba", 4), ("lrubx", 4), ("lrulam", 4), ("lrunorm", 4)):
            names.append(("%s%d" % (n, l), k))
    names.append(("fnorm", 16))
    off = {}
    o = 0
    for n, k in names:
        off[n] = (o, k)
        o += k
    return off, o


NCONST = 1152


def _consts():
    c = np.zeros((128, NCONST), np.float32)
    c[:, 0:128] = np.eye(128)
    c[:, 128:256] = np.triu(np.ones((128, 128)))
    su = np.triu(np.ones((64, 64)), 1)
    iu = np.triu(np.ones((64, 64)), 0)
    c[:, 256:320] = np.concatenate([su, su], 0)
    c[:, 320:384] = np.concatenate([iu, iu], 0)
    sl = np.tril(np.ones((64, 64)), -1)
    c[:, 384:448] = np.concatenate([sl, sl], 0)
    c[:, 448:512] = np.concatenate([np.eye(64), np.eye(64)], 0)
    p = np.arange(128)
    bd = np.zeros((128, 32, 4))
    bd[p, p // 4, :] = 1.0
    c[:, 512:640] = bd.reshape(128, 128)
    for h in range(4):
        c[h, 640 + h * 128:640 + (h + 1) * 128] = 1.0
    return c


def _fm(vec):
    vec = np.asarray(vec, np.float32).reshape(-1)
    return np.ascontiguousarray(vec.reshape(-1, 128).T)


def _pack(inp, depth):
    off, n = _pv_layout(depth)
    pv = np.zeros((128, n), np.float32)

    def put(name, arr):
        o, k = off[name]
        assert arr.shape == (128, k), (name, arr.shape, k)
        pv[:, o:o + k] = arr

    ps = np.zeros((96, 5 * depth), np.float32)
    for l in range(depth):
        for sub in range(3):
            put("gain%d%d" % (l, sub), _fm(inp["norm_gain"][l, sub]))
        put("modb%d" % l, _fm(inp["mod_b"][l]))
        put("hglog%d" % l, _fm(inp["hg_lb_logits"][l]))
        put("hgnorm%d" % l, _fm(inp["hg_norm"][l]))
        mu = np.asarray(inp["rw_mu"][l], np.float32)
        put("mur%d" % l, _fm(mu[0:512]))
        put("muk%d" % l, _fm(mu[512:1024]))
        put("muv%d" % l, _fm(mu[1024:1536]))
        ps[0:32, 5 * l + 0] = mu[1536:1568]
        ps[0:32, 5 * l + 1] = mu[1568:1600]
        ps[0:96, 5 * l + 2] = mu[1600:1696]
        ps[0:4, 5 * l + 3] = inp["ml_i_b"][l]
        ps[0:4, 5 * l + 4] = inp["ml_f_b"][l]
        for nm, key in (("w0", "rw_w0"), ("a0", "rw_a0"), ("kk", "rw_kk"), ("ka", "rw_ka"), ("rk", "rw_rk"),
                        ("lnw", "rw_ln_w"), ("lnb", "rw_ln_b"), ("mlcb", "ml_conv_b"), ("mlnorm", "ml_norm"),
                        ("mlskip", "ml_skip"), ("lrucb", "lru_conv_b"), ("lruba", "lru_ba"), ("lrubx", "lru_bx"),
                        ("lrulam", "lru_lambda"), ("lrunorm", "lru_norm")):
            put("%s%d" % (nm, l), _fm(inp[key][l]))
        for nm, key in (("mlcw", "ml_conv_w"), ("lrucw", "lru_conv_w")):
            cw = np.asarray(inp[key][l], np.float32)
            put("%s%d" % (nm, l), np.ascontiguousarray(cw.reshape(4, 4, 128).transpose(2, 1, 0)).reshape(128, 16))
        for nm, key in (("mlwq", "ml_wq"), ("mlwk", "ml_wk")):
            wq = np.asarray(inp[key][l], np.float32)
            put("%s%d" % (nm, l), np.ascontiguousarray(wq.reshape(4, 128, 4).transpose(1, 0, 2)).reshape(128, 16))
    put("fnorm", _fm(inp["final_norm"]))
    return pv, ps


def build(NT, DEPTH=2, mixers=(1, 1, 1, 1)):
    T = NT * TT
    nc = bass.Bass("TRN2", target_bir_lowering=False)
    es = ExitStack()
    pvoff, npv = _pv_layout(DEPTH)

    def dram(name, shape, dt, kind):
        h = nc.dram_tensor(name, list(shape), dt, kind=kind)
        return Buf(h.ap(), name, shape, 4 if dt == F32 else 2, dram=True)

    x_d = dram("x", [T, D], F32, "ExternalInput")
    out_d = dram("out", [T, D], F32, "ExternalOutput")
    cT_d = dram("cT", [128, 16], F32, "ExternalInput")
    pv_d = dram("pv", [128, npv], F32, "ExternalInput")
    ps_d = dram("ps", [96, 5 * DEPTH], F32, "ExternalInput")
    cst_d = dram("consts", [128, NCONST], F32, "ExternalInput")
    modw_d = dram("mod_w", [DEPTH, D, 9 * D], F32, "ExternalInput")
    w1_d = dram("ffn_w1", [DEPTH, 2, D, DFF], F32, "ExternalInput")
    w3_d = dram("ffn_w3", [DEPTH, 2, D, DFF], F32, "ExternalInput")
    w2_d = dram("ffn_w2", [DEPTH, 2, DFF, D], F32, "ExternalInput")
    win_d = dram("w_in", [DEPTH, D, DIN], F32, "ExternalInput")
    wout_d = dram("w_out", [DEPTH, D, D], F32, "ExternalInput")
    rww2_d = dram("rw_w2", [DEPTH, 32, 512], F32, "ExternalInput")
    rwa2_d = dram("rw_a2", [DEPTH, 32, 512], F32, "ExternalInput")
    rwg2_d = dram("rw_g2", [DEPTH, 96, 512], F32, "ExternalInput")
    lwa_d = dram("lru_wa", [DEPTH, 4, 128, 128], F32, "ExternalInput")
    lwx_d = dram("lru_wx", [DEPTH, 4, 128, 128], F32, "ExternalInput")
    w1_b = dram("w1b", [DEPTH, 2, D, DFF], BF16, "Internal")
    w3_b = dram("w3b", [DEPTH, 2, D, DFF], BF16, "Internal")
    w2_b = dram("w2b", [DEPTH, 2, DFF, D], BF16, "Internal")
    win_b = dram("winb", [DEPTH, D, DIN], BF16, "Internal")
    wout_b = dram("woutb", [DEPTH, D, D], BF16, "Internal")

    S = Sched(nc, es)

    def sbt(name, shape, dt=F32):
        t = es.enter_context(nc.sbuf_tensor(name, list(shape), dt))
        ap = t[tuple(slice(None) for _ in shape)]
        return Buf(ap, name, shape, 4 if dt == F32 else 2)

    PS = []
    for i in range(8):
        t = es.enter_context(nc.psum_tensor("ps%d" % i, [128, 512], F32))
        PS.append(Buf(t[:, :], "ps%d" % i, [128, 512], 4))

    xT = sbt("xT", [128, NFC, TT])
    hT = sbt("hT", [128, NFC, TT], BF16)
    BIGB = 73728
    big_t = es.enter_context(nc.sbuf_tensor("BIG", [128, BIGB // 4], F32))
    big_ap = big_t[:, :]

    def carve(off, shape, dt=F32):
        esz = 4 if dt == F32 else 2
        n = int(np.prod(shape[1:])) * esz
        assert off % 4 == 0 and off + n <= BIGB, (off, shape)
        ap = big_ap[:, off // 4:(off + n) // 4]
        if dt != F32:
            ap = ap.bitcast(dt)
        if len(shape) == 3:
            ap = ap.rearrange("p (a b) -> p a b", a=shape[1])
        elif len(shape) == 4:
            ap = ap.rearrange("p (a b c) -> p a b c", a=shape[1], b=shape[2])
        return Buf(ap, "BIG", shape, esz, base=off)

    WB = [sbt("wb%d" % i, [128, NFC, 256], BF16) for i in range(4)]
    cst = sbt("cst", [128, NCONST])
    pv = sbt("pvs", [128, npv])
    psm = sbt("psm", [96, 5 * DEPTH])
    dv = sbt("dv", [128, DEPTH * 3 * 48 + DEPTH * 40 + 16])
    modT = sbt("modT", [128, DEPTH, 144])
    TS = [sbt("tmp%d" % i, [128, TT]) for i in range(6)]
    RSTD = sbt("rstd", [128, TT])
    hgS = sbt("hgS", [128, DEPTH, 4, 128])
    mlS = sbt("mlS", [128, DEPTH, 4, 256])
    rwS = sbt("rwS", [128, DEPTH, 4, 64])
    stv = sbt("stv", [128, DEPTH, 64])
    mixw = sbt("mixw", [128, 2560])
    print("SBUF bytes remaining:", nc.sbuf_bytes_remaining)

    ident = cst[:, 0:128]
    triu128 = cst[:, 128:256]
    ones_t = sbt("ones", [128, 128])
    bones_t = sbt("bones", [128, 128])
    kc = sbt("kconst", [128, 8])

    def P(name, j=None):
        o, k = pvoff[name]
        return pv[:, o:o + k] if j is None else pv[:, o + j:o + j + 1]

    S.dma("sp", cst.all(), cst_d.all())
    S.dma("sp", pv.all(), pv_d.all())
    S.dma("sp", psm.all(), ps_d.all())
    S.memset(ones_t.all(), 1.0)
    S.memset(bones_t.all(), 0.0)
    S.memset(bones_t[0:64, 0:64], 1.0)
    S.memset(bones_t[64:128, 64:128], 1.0)
    for j, val in enumerate((1e-6, 1.0, 64e-5, 1e-12, 0.0)):
        S.memset(kc[:, j:j + 1], val)
    EPS6, ONE, EPSLN, EPS12, ZERO = (kc[:, j:j + 1] for j in range(5))
    S.memset(hgS.all(), 0.0)
    S.memset(mlS.all(), 0.0)
    S.memset(rwS.all(), 0.0)
    S.memset(stv.all(), 0.0)

    def cast_mat(dst, src, l_idx, rows, cols, run):
        a = cols // run
        assert a * run == cols
        for r0 in range(0, rows, 256):
            idx = tuple(l_idx) + (slice(r0, r0 + 256),)
            sv = src[idx]
            dv_ = dst[idx]
            S.dma("pool", dv_.m(lambda ap: ap.rearrange("r (a b) -> r a b", b=run)),
                  sv.m(lambda ap: ap.rearrange("r (a b) -> r a b", b=run)))

    for l in range(DEPTH):
        for f in range(2):
            if f == 1:
                cast_mat(win_b, win_d, (l,), D, DIN, 1578)
                cast_mat(wout_b, wout_d, (l,), D, D, 2048)
            cast_mat(w1_b, w1_d, (l, f), D, DFF, 1408)
            cast_mat(w3_b, w3_d, (l, f), D, DFF, 1408)
            cast_mat(w2_b, w2_d, (l, f), DFF, D, 2048)

    DVO = {}
    dvo = [0]

    def dvalloc(name, k):
        DVO[name] = (dvo[0], k)
        dvo[0] += k
        assert dvo[0] <= dv.shape[1]

    def DV(name, j=None):
        o, k = DVO[name]
        return dv[:, o:o + k] if j is None else dv[:, o + j:o + j + 1]

    cact = sbt("cact", [128, 16])
    S.dma("sp", cact.all(), cT_d.all())
    S.act(cact.all(), cact.all(), AF.Silu)

    for l in range(DEPTH):
        for n_, k in (("lb", 4), ("omlb", 4), ("ommur", 4), ("ommuk", 4), ("ommuv", 4), ("cneg", 4), ("cneg2", 4),
                      ("ommus", 4)):
            dvalloc("%s%d" % (n_, l), k)
        if l == 0:
            S.memset(DV("lb0"), 0.0)
        else:
            S.tt(DV("lb%d" % l), P("hglog1"), P("hglog0"), ALU.subtract)
            S.act(DV("lb%d" % l), DV("lb%d" % l), AF.Sigmoid)
        S.ts(DV("omlb%d" % l), DV("lb%d" % l), -1.0, ALU.mult, 1.0, ALU.add)
        for a_, b_ in (("ommur", "mur"), ("ommuk", "muk"), ("ommuv", "muv")):
            S.ts(DV("%s%d" % (a_, l)), P("%s%d" % (b_, l)), -1.0, ALU.mult, 1.0, ALU.add)
        o_, _ = DVO["ommus%d" % l]
        S.ts(dv[0:96, o_:o_ + 3], psm[0:96, 5 * l:5 * l + 3], -1.0, ALU.mult, 1.0, ALU.add)
        S.act(DV("cneg%d" % l), P("lrulam%d" % l), AF.Exp, scale=-1.0)
        S.act(DV("cneg%d" % l), DV("cneg%d" % l), AF.Ln, bias=ONE)
        S.ts(DV("cneg2%d" % l), DV("cneg%d" % l), -16.0, ALU.mult)
        S.ts(DV("cneg%d" % l), DV("cneg%d" % l), -8.0, ALU.mult)

    MW = [carve(0, [128, 16, 512]), carve(32768, [128, 16, 512])]
    for l in range(DEPTH):
        src = modw_d.ap[l].rearrange("(kc p) n -> p kc n", p=128)
        for cb in range(36):
            mw = MW[cb % 2]
            S.dma("sp", mw.all(), V(src[:, :, cb * 512:(cb + 1) * 512], "mod_w", 0, 1))
            for m_ in range(4):
                col = cb * 4 + m_
                for k_ in range(16):
                    S.mm(PS[7][:, col:col + 1], mw[:, k_, m_ * 128:(m_ + 1) * 128], cact[:, k_:k_ + 1],
                         start=(k_ == 0), stop=(k_ == 15))
        S.tt(modT[:, l, :], PS[7][:, 0:144], P("modb%d" % l), ALU.add)
        for sub in range(3):
            for n_ in ("A", "G"):
                dvalloc("%s%d%d" % (n_, l, sub), 16)
            sh = modT[:, l, (sub * 3 + 0) * 16:(sub * 3 + 1) * 16]
            scl = modT[:, l, (sub * 3 + 1) * 16:(sub * 3 + 2) * 16]
            gat = modT[:, l, (sub * 3 + 2) * 16:(sub * 3 + 3) * 16]
            S.stt(DV("A%d%d" % (l, sub)), scl, 1.0, P("gain%d%d" % (l, sub)), ALU.add, ALU.mult)
            S.ts(DV("G%d%d" % (l, sub)), gat, 1.0, ALU.add, (1.0 if sub == 1 else 0.5), ALU.mult)

    def SHIFT(l, sub, fc):
        return modT[:, l, (sub * 3) * 16 + fc:(sub * 3) * 16 + fc + 1]

    XIN = [carve(40960, [128, D]), carve(49152, [128, D])]
    wb_rr = [0]

    def next_wb():
        b = WB[wb_rr[0] % 4]
        wb_rr[0] += 1
        return b

    def rstd_of(chunks, n_feat, eps_v, lhs_ones):
        n = len(chunks)
        for i, ch in enumerate(chunks):
            t = TS[i % 2]
            S.act(t.all(), ch, AF.Square)
            S.mm(PS[6].all(), lhs_ones, t.all(), start=(i == 0), stop=(i == n - 1))
        S.act(RSTD.all(), PS[6].all(), AF.Sqrt, bias=eps_v, scale=1.0 / n_feat)
        S.recip(RSTD.all(), RSTD.all())

    def modnorm(l, sub):
        rstd_of([xT[:, fc, :] for fc in range(NFC)], D, EPS6, ones_t.all())
        for fc in range(NFC):
            t = TS[2 + fc % 2]
            S.stt(t.all(), xT[:, fc, :], DV("A%d%d" % (l, sub), fc), RSTD.all(), ALU.mult, ALU.mult)
            S.act(hT[:, fc, :], t.all(), AF.Identity, bias=SHIFT(l, sub, fc))

    def wload(dst, srcbuf, lidx, row_lo, nrows_chunks, col_lo, ncols):
        src = srcbuf.ap[lidx] if len(lidx) == 1 else srcbuf.ap[lidx[0], lidx[1]]
        src = src[row_lo:row_lo + nrows_chunks * 128, col_lo:col_lo + ncols].rearrange("(kc p) n -> p kc n", p=128)
        nb = srcbuf.strides[len(lidx) - 1]
        base = sum(i * srcbuf.strides[k] for k, i in enumerate(lidx))
        S.dma("sp", dst, V(src, srcbuf.tid, base, base + nb))

    def resid_update(l, sub, fc, psv):
        S.stt(xT[:, fc, :], psv, DV("G%d%d" % (l, sub), fc), xT[:, fc, :], ALU.mult, ALU.add)

    def ffn(l, f):
        sub = 0 if f == 0 else 2
        modnorm(l, sub)
        gT = carve(0, [128, 22, TT], BF16)
        W2B = [carve(22528, [128, 22, 256], BF16), carve(22528 + 11264, [128, 22, 256], BF16)]
        for half in range(2):
            for grp in range(11):
                ff0 = half * 2816 + grp * 256
                b1 = next_wb()
                b3 = next_wb()
                wload(b1.all(), w1_b, (l, f), 0, 16, ff0, 256)
                wload(b3.all(), w3_b, (l, f), 0, 16, ff0, 256)
                for s_ in range(2):
                    ci = grp * 2 + s_
                    pa = PS[(ci % 2) * 2]
                    pb = PS[(ci % 2) * 2 + 1]
                    for k_ in range(16):
                        S.mm(pa.all(), b1[:, k_, s_ * 128:(s_ + 1) * 128], hT[:, k_, :], start=(k_ == 0), stop=(k_ == 15))
                    for k_ in range(16):
                        S.mm(pb.all(), b3[:, k_, s_ * 128:(s_ + 1) * 128], hT[:, k_, :], start=(k_ == 0), stop=(k_ == 15))
                    t = TS[4 + ci % 2]
                    S.act(t.all(), pa.all(), AF.Silu)
                    S.tt(gT[:, ci, :], pb.all(), t.all(), ALU.mult)
            for pr in range(8):
                wb2 = W2B[pr % 2]
                src = w2_b.ap[l, f][half * 2816:(half + 1) * 2816, pr * 256:(pr + 1) * 256].rearrange(
                    "(kc p) n -> p kc n", p=128)
                nb = w2_b.strides[1]
                base = l * w2_b.strides[0] + f * w2_b.strides[1]
                S.dma("sp", wb2.all(), V(src, w2_b.tid, base, base + nb))
                for s_ in range(2):
                    fc = pr * 2 + s_
                    po = PS[4 + fc % 2]
                    for k_ in range(22):
                        S.mm(po.all(), wb2[:, k_, s_ * 128:(s_ + 1) * 128], gT[:, k_, :], start=(k_ == 0), stop=(k_ == 21))
                    resid_update(l, sub, fc, po.all())

    YT_OFF = 57344
    ups_rr = [0]

    def ugemm(l, col0, ncols):
        b = next_wb()
        wload(b[:, :, 0:ncols], win_b, (l,), 0, 16, col0, ncols)
        p = PS[ups_rr[0] % 3]
        ups_rr[0] += 1
        for k_ in range(16):
            S.mm(p[0:ncols, :], b[:, k_, 0:ncols], hT[:, k_, :], start=(k_ == 0), stop=(k_ == 15))
        return p[0:ncols, :]

    def SL(i):
        return carve(i * 2048, [128, TT])

    def conv4(l, cwname, cbname, c, xbuf, out):
        o, _ = pvoff["%s%d" % (cwname, l)]
        w = [pv[:, o + c * 4 + j:o + c * 4 + j + 1] for j in range(4)]
        S.ts(out, xbuf[:, 3:515], w[3], ALU.mult, P("%s%d" % (cbname, l), c), ALU.add)
        for j in range(3):
            S.stt(out, xbuf[:, j:j + 512], w[j], out, ALU.mult, ALU.add)

    def head_norm_out(zs, n_feat, lhs_ones, eps_v):
        rstd_of(zs, n_feat, eps_v, lhs_ones)

    def mixer(l, yT):
        sub = 1
        modnorm(l, sub)
        o_ps = 5 * l
        w2m = Buf(mixw.ap[0:32, 0:512], "mixw", [32, 512], 4, base=0)
        a2m = Buf(mixw.ap[0:32, 512:1024], "mixw", [32, 512], 4, base=2048)
        g2m = Buf(mixw.ap[0:96, 1024:1536], "mixw", [96, 512], 4, base=4096)
        wam = Buf(mixw.ap[:, 1536:2048].rearrange("p (a b) -> p a b", a=4), "mixw", [128, 4, 128], 4, base=6144)
        wxm = Buf(mixw.ap[:, 2048:2560].rearrange("p (a b) -> p a b", a=4), "mixw", [128, 4, 128], 4, base=8192)
        S.dma("sp", w2m.all(), V(rww2_d.ap[l], "rw_w2", 0, 1))
        S.dma("sp", a2m.all(), V(rwa2_d.ap[l], "rw_a2", 0, 1))
        S.dma("sp", g2m.all(), V(rwg2_d.ap[l], "rw_g2", 0, 1))
        S.dma("sp", wam.all(), V(lwa_d.ap[l].rearrange("n i j -> i n j"), "lru_wa", 0, 1))
        S.dma("sp", wxm.all(), V(lwx_d.ap[l].rearrange("n i j -> i n j"), "lru_wx", 0, 1))

        for c in range(4):
            if not mixers[0]:
                S.memset(yT[:, c, :], 0.0)
                continue
            q, fg, kk_, vv, gs, Bc, qt, kt, qh, kh = (SL(i) for i in range(10))
            small = SL(10)
            S.memset(small.all(), 0.0)
            S.memset(carve(11 * 2048, [128, 768]).all(), 0.0)
            pq = ugemm(l, C_HG + c * 128, 128)
            S.act(q.all(), pq, AF.Silu)
            pf = ugemm(l, C_HG + 512 + c * 128, 128)
            S.act(fg.all(), pf, AF.Sigmoid)
            S.ts(fg.all(), fg.all(), DV("omlb%d" % l, c), ALU.mult, DV("lb%d" % l, c), ALU.add)
            S.ts(kk_.all(), fg.all(), -1.0, ALU.mult, 1.0, ALU.add)
            S.act(fg.all(), fg.all(), AF.Ln)
            pi = ugemm(l, C_HG + 1024 + c * 128, 128)
            S.cp(vv.all(), pi, eng="act")
            pg = ugemm(l, C_HG + 1536 + c * 128, 128)
            S.act(gs.all(), pg, AF.Silu)
            for b_ in range(8):
                sl_ = slice(b_ * 64, (b_ + 1) * 64)
                S.scan(Bc[:, sl_], ones_t[:, 0:64], fg[:, sl_], 0.0, ALU.mult, ALU.add)
            for b_ in range(8):
                sl_ = slice(b_ * 64, (b_ + 1) * 64)
                mid = Bc[:, b_ * 64 + 31:b_ * 64 + 32]
                end = Bc[:, b_ * 64 + 63:b_ * 64 + 64]
                S.ts(small[:, b_:b_ + 1], mid, -1.0, ALU.mult)
                S.act(qt[:, sl_], Bc[:, sl_], AF.Exp, bias=small[:, b_:b_ + 1])
                S.act(kt[:, sl_], Bc[:, sl_], AF.Exp, bias=mid, scale=-1.0)
                S.act(kh[:, sl_], Bc[:, sl_], AF.Exp, bias=end, scale=-1.0)
                S.act(small[:, 8 + b_:9 + b_], end, AF.Exp)
            S.act(qh.all(), Bc.all(), AF.Exp)
            S.tt(qt.all(), qt.all(), q.all(), ALU.mult)
            S.tt(kt.all(), kt.all(), kk_.all(), ALU.mult)
            S.tt(qh.all(), qh.all(), q.all(), ALU.mult)
            S.tt(kh.all(), kh.all(), kk_.all(), ALU.mult)
            Sst = hgS[:, l, c, :]
            blkbuf = carve(11 * 2048, [128, 2, 3, 128])
            ktx, vvx, khx = carve(7 * 2048, [128, 576]), carve(3 * 2048, [128, 576]), carve(9 * 2048, [128, 576])
            STG = int(_os.environ.get("HG_STAGE", "9"))
            for b_ in range(8 if STG > 1 else 0):
                sl_ = slice(b_ * 64, (b_ + 1) * 64)
                slx = slice(b_ * 64, b_ * 64 + 128)
                r_ = b_ % 2
                pt = PS[3]
                S.mm(pt[:, 0:64], ktx[:, slx], qt[:, sl_])
                S.tt(blkbuf[0:64, r_, 0, 0:64], pt[0:64, 0:64], cst[0:64, 320:384], ALU.mult)
                if STG <= 2:
                    continue
                S.tr(pt[:, 128:256], vvx[:, slx], ident)
                S.tr(pt[:, 256:384], khx[:, slx], ident)
                S.cp(blkbuf[0:64, r_, 1, :], pt[0:64, 128:256], eng="act")
                S.cp(blkbuf[0:64, r_, 2, :], pt[0:64, 256:384], eng="act")
                if STG <= 3:
                    continue
                po = PS[4]
                S.mm(po[:, sl_], blkbuf[:, r_, 1, :], blkbuf[:, r_, 0, 0:64], start=True, stop=False)
                S.mm(po[:, sl_], Sst, qh[:, sl_], start=False, stop=True)
                if STG <= 4:
                    continue
                S.mm(PS[5][:, 0:128], blkbuf[:, r_, 2, :], blkbuf[:, r_, 1, :])
                S.stt(Sst, Sst, small[:, 8 + b_:9 + b_], PS[5][:, 0:128], ALU.mult, ALU.add)
            osb = q
            if STG <= 3:
                S.cp(yT[:, c, :], qt.all())
                continue
            S.cp(osb.all(), PS[4].all(), eng="act")
            rstd_of([osb.all()], 128, EPS6, ones_t.all())
            S.stt(osb.all(), osb.all(), P("hgnorm%d" % l, c), RSTD.all(), ALU.mult, ALU.mult)
            S.tt(yT[:, c, :], osb.all(), gs.all(), ALU.mult)

        zs = [SL(i) for i in range(4)]
        for c in range(4):
            if not mixers[3]:
                S.memset(yT[:, 12 + c, :], 0.0)
                continue
            xbuf = carve(4 * 2048, [128, 516])
            xc, rg, ig, aa, a2, th, hb = (SL(i) for i in range(6, 13))
            px = ugemm(l, C_LRU + c * 128, 128)
            S.cp(xbuf[:, 0:3], stv[:, l, c * 3:c * 3 + 3])
            S.cp(xbuf[:, 3:515], px, eng="act")
            S.cp(stv[:, l, c * 3:c * 3 + 3], xbuf[:, 512:515])
            conv4(l, "lrucw", "lrucb", c, xbuf, xc.all())
            S.mm(PS[3].all(), wam[:, c, :], xc.all())
            S.mm(PS[4].all(), wxm[:, c, :], xc.all())
            S.act(rg.all(), PS[3].all(), AF.Sigmoid, bias=P("lruba%d" % l, c))
            S.act(ig.all(), PS[4].all(), AF.Sigmoid, bias=P("lrubx%d" % l, c))
            S.act(aa.all(), rg.all(), AF.Exp, scale=DV("cneg%d" % l, c))
            S.act(a2.all(), rg.all(), AF.Exp, scale=DV("cneg2%d" % l, c))
            S.act(th.all(), rg.all(), AF.Tanh, scale=DV("cneg%d" % l, c))
            S.stt(a2.all(), a2.all(), 1.0, th.all(), ALU.add, ALU.mult)
            S.act(a2.all(), a2.all(), AF.Sqrt, scale=-1.0)
            S.tt(ig.all(), ig.all(), xc.all(), ALU.mult)
            S.tt(ig.all(), ig.all(), a2.all(), ALU.mult)
            S.scan(hb.all(), aa.all(), ig.all(), stv[:, l, 12 + c:13 + c], ALU.mult, ALU.add)
            S.cp(stv[:, l, 12 + c:13 + c], hb[:, 511:512])
            pgb = ugemm(l, C_LRU + 512 + c * 128, 128)
            S.act(rg.all(), pgb, AF.Square)
            S.ts(rg.all(), rg.all(), 0.044715, ALU.mult, 1.0, ALU.add)
            S.tt(rg.all(), rg.all(), pgb, ALU.mult)
            S.act(rg.all(), rg.all(), AF.Sigmoid, scale=1.5957691216057308)
            S.tt(rg.all(), rg.all(), pgb, ALU.mult)
            S.tt(zs[c].all(), rg.all(), hb.all(), ALU.mult)
        if mixers[3]:
            rstd_of([z.all() for z in zs], 512, EPS6, ones_t.all())
            for c in range(4):
                S.stt(yT[:, 12 + c, :], zs[c].all(), P("lrunorm%d" % l, c), RSTD.all(), ALU.mult, ALU.mult)

        if not mixers[2]:
            for c in range(4):
                S.memset(yT[:, 8 + c, :], 0.0)
        else:
            mlstm(l, yT)
        if not mixers[1]:
            for c in range(4):
                S.memset(yT[:, 4 + c, :], 0.0)
        else:
            rwkv(l, yT, w2m, a2m, g2m)

        for pr in range(8):
            b = next_wb()
            wload(b.all(), wout_b, (l,), 0, 16, pr * 256, 256)
            for s_ in range(2):
                fc = pr * 2 + s_
                po = PS[4 + fc % 2]
                for k_ in range(16):
                    S.mm(po.all(), b[:, k_, s_ * 128:(s_ + 1) * 128], yT[:, k_, :], start=(k_ == 0), stop=(k_ == 15))
                resid_update(l, sub, fc, po.all())

    def mlstm(l, yT):
        xcs = carve(0, [128, 4, TT])
        g4 = carve(4 * 2048, [128, 8, TT])
        i_pre, lf, Bg, dd, Mg, negM, negMB = (g4[0:4, j, :] for j in range(7))
        tmp4 = g4[0:4, 7, :]
        pib = ugemm(l, C_ML + 1536, 4)
        S.act(i_pre, pib, AF.Identity, bias=psm[0:4, 5 * l + 3:5 * l + 4])
        pfb = ugemm(l, C_ML + 1540, 4)
        S.act(lf, pfb, AF.Identity, bias=psm[0:4, 5 * l + 4:5 * l + 5])
        S.act(tmp4, lf, AF.Abs)
        S.act(tmp4, tmp4, AF.Exp, scale=-1.0)
        S.act(tmp4, tmp4, AF.Ln, bias=kc[0:4, 1:2])
        S.ts(lf, lf, 0.0, ALU.min)
        S.tt(lf, lf, tmp4, ALU.subtract)
        onesrow = carve(12 * 2048, [128, TT])
        S.memset(onesrow[0:4, :], 1.0)
        S.scan(Bg, onesrow[0:4, :], lf, stv[0:4, l, 44:45], ALU.mult, ALU.add)
        S.cp(stv[0:4, l, 44:45], g4[0:4, 2, 511:512])
        S.tt(dd, i_pre, Bg, ALU.subtract)
        S.scan(Mg, onesrow[0:4, :], dd, stv[0:4, l, 43:44], ALU.mult, ALU.max)
        S.cp(stv[0:4, l, 43:44], g4[0:4, 4, 511:512])
        S.ts(negM, Mg, -1.0, ALU.mult)
        S.tt(negMB, negM, Bg, ALU.subtract)
        dT = carve(13 * 2048, [128, 4, 4])
        for b_ in range(4):
            S.tr(PS[3][:, b_ * 4:b_ * 4 + 4], g4[0:4, 3, b_ * 128:(b_ + 1) * 128], cst[0:4, 0:4])
        S.cp(dT.all(), PS[3][:, 0:16].m(lambda ap: ap.rearrange("p (a b) -> p a b", a=4)))
        xbuf = carve(13 * 2048 + 256, [128, 516])
        for c in range(4):
            px = ugemm(l, C_ML + c * 128, 128)
            S.cp(xbuf[:, 0:3], stv[:, l, 16 + c * 3:16 + c * 3 + 3])
            S.cp(xbuf[:, 3:515], px, eng="act")
            S.cp(stv[:, l, 16 + c * 3:16 + c * 3 + 3], xbuf[:, 512:515])
            conv4(l, "mlcw", "mlcb", c, xbuf, xcs[:, c, :])
            S.act(xcs[:, c, :], xcs[:, c, :], AF.Silu)
        wbd = carve(15 * 2048, [128, 2, 128])
        qT, kT, vS, hfull, osig = (SL(i) for i in range(16, 21))
        blk = carve(21 * 2048, [128, 2, 8, 128])
        small = carve(25 * 2048 + 0, [128, 64])
        sel = cst[0:4, 640:1152].m(lambda ap: ap.rearrange("p (h m) -> p h m", h=4))
        for h in range(4):
            c = h
            for nm_, dst in (("mlwq", 0), ("mlwk", 1)):
                o, _ = pvoff["%s%d" % (nm_, l)]
                rows = pv[:, o + c * 4:o + c * 4 + 4]
                S.tt(wbd[:, dst, :].m(lambda ap: ap.rearrange("p (n i) -> p n i", i=4)),
                     cst[:, 512:640].m(lambda ap: ap.rearrange("p (n i) -> p n i", i=4)),
                     rows.m(lambda ap: ap.unsqueeze(1).to_broadcast([128, 32, 4])), ALU.mult)
            S.mm(PS[3].all(), wbd[:, 0, :], xcs[:, c, :])
            S.mm(PS[4].all(), wbd[:, 1, :], xcs[:, c, :])
            S.act(qT.all(), PS[3].all(), AF.Copy, scale=128.0 ** -0.5)
            S.cp(kT.all(), PS[4].all(), eng="act")
            pv_ = ugemm(l, C_ML + 512 + c * 128, 128)
            S.cp(vS.all(), pv_, eng="act")
            po_ = ugemm(l, C_ML + 1024 + c * 128, 128)
            S.act(osig.all(), po_, AF.Sigmoid)
            selh = V(sel.ap[:, h, :], "cst", sel.lo, sel.hi)
            S.mm(PS[5].all(), selh, negM)
            S.mm(PS[6].all(), selh, negMB)
            S.cp(small[:, h * 5:h * 5 + 1], stv[:, l, 48 + h:49 + h])
            for j in range(4):
                S.cp(small[:, h * 5 + 1 + j:h * 5 + 2 + j], PS[5][:, j * 128 + 127:j * 128 + 128])
            S.cp(stv[:, l, 48 + h:49 + h], small[:, h * 5 + 4:h * 5 + 5])
            S.ts(small[:, 20 + h * 5:25 + h * 5], small[:, h * 5:h * 5 + 5], -1.0, ALU.mult)
            Sh = mlS[:, l, h, :]
            for j in range(4):
                sl_ = slice(j * 128, (j + 1) * 128)
                r_ = j % 2
                PT, wT, qw, ktw, em, misc = (blk[:, r_, i, :] for i in (0, 1, 2, 3, 6, 7))
                Vaug = V(blk.ap[:, r_, 4:6, :].rearrange("p a b -> p (a b)"), "BIG",
                         blk[:, r_, 4:6, :].lo, blk[:, r_, 4:6, :].hi)
                Vv = blk[:, r_, 4, :]
                Vo = blk[:, r_, 5, :]
                S.memset(Vo, 1.0)
                S.mm(PS[3][:, 0:128], kT[:, sl_], qT[:, sl_])
                S.tr(PS[3][:, 128:256], vS[:, sl_], ident)
                S.tr(PS[3][:, 256:384], kT[:, sl_], ident)
                S.act(wT, PS[5][:, sl_], AF.Exp, bias=dT[:, j, h:h + 1])
                S.tt(wT, wT, triu128, ALU.mult)
                S.tt(PT, PS[3][:, 0:128], wT, ALU.mult)
                S.cp(Vv, PS[3][:, 128:256], eng="act")
                S.act(qw, PS[5][:, sl_], AF.Exp, bias=small[:, 20 + h * 5 + j:21 + h * 5 + j])
                S.tt(qw, qw, qT[:, sl_], ALU.mult)
                S.act(small[:, 40 + h * 4 + j:41 + h * 4 + j], dT[:, j, h:h + 1], AF.Exp,
                      bias=small[:, h * 5 + 1 + j:h * 5 + 2 + j])
                S.act(small[:, 56 + r_:57 + r_], small[:, 20 + h * 5 + j:21 + h * 5 + j], AF.Exp,
                      bias=small[:, h * 5 + 1 + j:h * 5 + 2 + j])
                S.ts(ktw, PS[3][:, 256:384], small[:, 40 + h * 4 + j:41 + h * 4 + j], ALU.mult)
                pn = PS[7]
                S.mm(pn[:, 0:128], Vv, PT, start=True, stop=False)
                S.mm(pn[:, 0:128], Sh.m(lambda ap: ap[:, 0:128]), qw, start=False, stop=True)
                S.mm(pn[:, 128:256], Vo, PT, start=True, stop=False)
                S.mm(pn[:, 128:256], Sh.m(lambda ap: ap[:, 128:256]), qw, start=False, stop=True)
                S.act(em, PS[6][:, sl_], AF.Exp)
                S.act(misc, pn[:, 128:256], AF.Abs)
                S.tt(em, misc, em, ALU.max)
                S.recip(em, em)
                S.tt(misc, pn[:, 0:128], em, ALU.mult)
                S.tt(hfull[:, sl_], misc, osig[:, sl_], ALU.mult)
                S.mm(PS[4][:, 0:256], ktw, Vaug)
                S.stt(Sh, Sh, small[:, 56 + r_:57 + r_], PS[4][:, 0:256], ALU.mult, ALU.add)
            rstd_of([hfull.all()], 128, EPS6, ones_t.all())
            S.stt(hfull.all(), hfull.all(), P("mlnorm%d" % l, c), RSTD.all(), ALU.mult, ALU.mult)
            S.stt(yT[:, 8 + c, :], xcs[:, c, :], P("mlskip%d" % l, c), hfull.all(), ALU.mult, ALU.add)

    def rwkv(l, yT, w2m, a2m, g2m):
        o_s, _ = DVO["ommus%d" % l]
        lr = carve(0, [128, 3, TT])
        ub = carve(3 * 2048, [128, 516])
        tmp = SL(27)

        def shifted(pu, rows, mu_v, omm_v, prev_v, out):
            S.cp(ub[0:rows, 0:1], prev_v)
            S.cp(ub[0:rows, 1:513], pu, eng="act")
            S.cp(prev_v, ub[0:rows, 512:513])
            S.ts(tmp[0:rows, :], ub[0:rows, 1:513], omm_v, ALU.mult)
            S.stt(out, ub[0:rows, 0:512], mu_v, tmp[0:rows, :], ALU.mult, ALU.add)

        pw = ugemm(l, C_RW + 1536, 32)
        shifted(pw, 32, psm[0:32, 5 * l:5 * l + 1], dv[0:32, o_s:o_s + 1], stv[0:32, l, 40:41], lr[0:32, 0, :])
        S.act(lr[0:32, 0, :], lr[0:32, 0, :], AF.Tanh)
        pa = ugemm(l, C_RW + 1568, 32)
        shifted(pa, 32, psm[0:32, 5 * l + 1:5 * l + 2], dv[0:32, o_s + 1:o_s + 2], stv[0:32, l, 41:42], lr[0:32, 1, :])
        pg = ugemm(l, C_RW + 1600, 96)
        shifted(pg, 96, psm[0:96, 5 * l + 2:5 * l + 3], dv[0:96, o_s + 2:o_s + 3], stv[0:96, l, 42:43], lr[0:96, 2, :])
        S.act(lr[0:96, 2, :], lr[0:96, 2, :], AF.Sigmoid)
        rwmask = cst[:, 256:384]
        maskSL = cst[:, 384:448]
        identd = cst[:, 448:512]
        for g in range(4):
            rs, ks, vs, sg, Ep, Em, aic, gate, bonus, ofull, kkn = (SL(i) for i in range(5, 16))
            AR = carve(16 * 2048, [128, 8, 2, 64])
            bt, kt = SL(18), SL(19)
            for (pc, mu_n, omm_n, pcol, dst) in ((0, "mur", "ommur", 28, rs), (512, "muk", "ommuk", 32, ks),
                                                 (1024, "muv", "ommuv", 36, vs)):
                pu = ugemm(l, C_RW + pc + g * 128, 128)
                shifted(pu, 128, P("%s%d" % (mu_n, l), g), DV("%s%d" % (omm_n, l), g),
                        stv[:, l, pcol + g:pcol + g + 1], dst.all())
            sl_g = slice(g * 128, (g + 1) * 128)
            S.mm(PS[3].all(), w2m[0:32, sl_g], lr[0:32, 0, :])
            S.act(sg.all(), PS[3].all(), AF.Sigmoid, bias=P("w0%d" % l, g))
            S.mm(PS[4].all(), a2m[0:32, sl_g], lr[0:32, 1, :])
            S.act(aic.all(), PS[4].all(), AF.Sigmoid, bias=P("a0%d" % l, g))
            S.mm(PS[5].all(), g2m[0:96, sl_g], lr[0:96, 2, :])
            S.cp(gate.all(), PS[5].all(), eng="act")
            S.ts(kkn.all(), ks.all(), P("kk%d" % l, g), ALU.mult)
            S.act(tmp.all(), kkn.all(), AF.Square)
            S.mm(PS[3].all(), bones_t.all(), tmp.all())
            S.act(tmp.all(), PS[3].all(), AF.Sqrt, bias=EPS12)
            S.ts(tmp.all(), tmp.all(), 1e-6, ALU.max)
            S.recip(tmp.all(), tmp.all())
            S.tt(kkn.all(), kkn.all(), tmp.all(), ALU.mult)
            S.ts(tmp.all(), aic.all(), -1.0, ALU.add, P("ka%d" % l, g), ALU.mult)
            S.stt(ks.all(), tmp.all(), 1.0, ks.all(), ALU.add, ALU.mult)
            S.stt(tmp.all(), rs.all(), P("rk%d" % l, g), ks.all(), ALU.mult, ALU.mult)
            S.mm(PS[4].all(), bones_t.all(), tmp.all())
            S.tt(bonus.all(), PS[4].all(), vs.all(), ALU.mult)
            Lp = tmp
            for b_ in range(8):
                sl_ = slice(b_ * 64, (b_ + 1) * 64)
                S.scan(Lp[:, sl_], ones_t[:, 0:64], sg[:, sl_], 0.0, ALU.mult, ALU.add)
            S.act(Ep.all(), Lp.all(), AF.Exp, scale=-EXPM05)
            S.act(Em.all(), Lp.all(), AF.Exp, scale=EXPM05)
            S.tt(Lp.all(), Lp.all(), sg.all(), ALU.subtract)
            S.act(Lp.all(), Lp.all(), AF.Exp, scale=-EXPM05)
            ar_a = V(AR.ap[:, :, 0, :], "BIG", AR.base, AR.base + AR.nbytes)
            ar_r = V(AR.ap[:, :, 1, :], "BIG", AR.base, AR.base + AR.nbytes)
            v3 = lambda vv_: vv_.m(lambda ap: ap.rearrange("p (b t) -> p b t", t=64))
            S.tt(v3(Lp.all()), v3(Lp.all()), v3(kkn.all()), ALU.mult)
            S.ts(ar_a, v3(Lp.all()), -1.0, ALU.mult)
            S.tt(ar_r, v3(rs.all()), v3(Ep.all()), ALU.mult)
            S.tt(bt.all(), kkn.all(), aic.all(), ALU.mult)
            S.tt(bt.all(), bt.all(), Em.all(), ALU.mult)
            S.tt(kt.all(), ks.all(), Em.all(), ALU.mult)
            St = rwS[:, l, g, :]
            BB = carve(20 * 2048, [128, 2, 14, 128])
            for b_ in range(8):
                sl_ = slice(b_ * 64, (b_ + 1) * 64)
                r_ = b_ % 2
                A1, A2, Nn, btok, ktok, vtok, RH, Us, Y0, Y1 = (BB[:, r_, i, :] for i in range(10))
                PQ = [BB[:, r_, 10, :], BB[:, r_, 11, :], BB[:, r_, 12, :], BB[:, r_, 13, :]]
                arb = V(AR.ap[:, b_, :, :].rearrange("p a t -> p (a t)"), "BIG", AR.base, AR.base + AR.nbytes)
                ara = V(AR.ap[:, b_, 0, :], "BIG", AR.base, AR.base + AR.nbytes)
                arr = V(AR.ap[:, b_, 1, :], "BIG", AR.base, AR.base + AR.nbytes)

                def hp(v_, hh, c0=None, c1=None):
                    return v_.m(lambda ap: ap[hh * 64:(hh + 1) * 64] if c0 is None else ap[hh * 64:(hh + 1) * 64, c0:c1])

                for hh in range(2):
                    S.mm(PS[3][hh * 64:(hh + 1) * 64, 0:128], hp(bt[:, sl_], hh), hp(arb, hh))
                    S.mm(PS[3][hh * 64:(hh + 1) * 64, 128:256], hp(kt[:, sl_], hh), hp(arb, hh))
                    S.mm(PS[3][hh * 64:(hh + 1) * 64, 256:320], hp(ara, hh), hp(bt[:, sl_], hh))
                dup = lambda v_: v_.m(lambda ap: ap.unsqueeze(1).to_broadcast([128, 2, 64]))
                DUP = carve((8 if r_ == 0 else 11) * 2048, [128, 4, 128])
                for i_, src_ in enumerate((bt, kt, vs)):
                    S.cp(DUP[:, i_, :].m(lambda ap: ap.rearrange("p (a b) -> p a b", a=2)), dup(src_[:, sl_]), eng="pool")
                    S.tr(PS[4][:, i_ * 128:(i_ + 1) * 128], DUP[:, i_, :], ident)
                S.tt(A1, PS[3][:, 0:128], rwmask, ALU.mult)
                S.tt(A2, PS[3][:, 128:256], rwmask, ALU.mult)
                S.tt(Nn.m(lambda ap: ap[:, 0:64]), PS[3][:, 256:320], maskSL, ALU.mult)
                S.cp(BB[:, r_, 3:6, :], PS[4][:, 0:384].m(lambda ap: ap.rearrange("p (a b) -> p a b", a=3)), eng="act")
                Pc = A1.m(lambda ap: ap[:, 0:64])
                Qc = Nn.m(lambda ap: ap[:, 0:64])
                S.tt(Y0.m(lambda ap: ap[:, 0:64]), Pc, identd, ALU.add)
                Yc = Y0.m(lambda ap: ap[:, 0:64])
                Yn = Y1.m(lambda ap: ap[:, 0:64])
                for lev in range(5):
                    pq = PQ[lev % 4] if lev < 4 else A2
                    pq = PQ[lev % 4]
                    pp = PS[5 + lev % 2]
                    for hh in range(2):
                        S.mm(pp[hh * 64:(hh + 1) * 64, 0:64], hp(Qc, hh), hp(Pc, hh))
                        S.mm(pp[hh * 64:(hh + 1) * 64, 64:128], hp(Pc, hh), hp(Qc, hh))
                    if lev == 4:
                        pass
                    S.cp(pq, pp[:, 0:128], eng="act")
                    Pc = pq.m(lambda ap: ap[:, 0:64])
                    Qc = pq.m(lambda ap: ap[:, 64:128])
                    py = PS[7]
                    for hh in range(2):
                        S.mm(py[hh * 64:(hh + 1) * 64, 0:64], hp(Qc, hh), hp(Yc, hh))
                    S.tt(Yn, py[:, 0:64], Yc, ALU.add)
                    Yc, Yn = Yn, Yc
                pr_ = PS[4]
                for hh in range(2):
                    o_ = pr_[hh * 64:(hh + 1) * 64, 384:448]
                    S.mm(o_, hp(ara, hh), hp(St, hh), start=True, stop=False)
                    S.mm(o_, hp(A2, hh, 0, 64), hp(vtok, hh, hh * 64, (hh + 1) * 64), start=False, stop=True)
                S.cp(RH.m(lambda ap: ap[:, 0:64]), pr_[:, 384:448])
                for hh in range(2):
                    S.mm(pr_[hh * 64:(hh + 1) * 64, 448:512], hp(Yc, hh), hp(RH, hh, 0, 64))
                S.cp(Us.m(lambda ap: ap[:, 0:64]), pr_[:, 448:512])
                po = PS[7]
                for hh in range(2):
                    o_ = po[hh * 64:(hh + 1) * 64, 64:128]
                    S.mm(o_, hp(St, hh), hp(arr, hh), start=True, stop=False)
                    S.mm(o_, hp(Us, hh, 0, 64), hp(A1, hh, 64, 128), start=False, stop=False)
                    S.mm(o_, hp(vtok, hh, hh * 64, (hh + 1) * 64), hp(A2, hh, 64, 128), start=False, stop=True)
                S.cp(ofull[:, sl_], po[:, 64:128], eng="act")
                pS = PS[7]
                for hh in range(2):
                    o_ = pS[hh * 64:(hh + 1) * 64, 128:192]
                    S.mm(o_, hp(btok, hh, hh * 64, (hh + 1) * 64), hp(Us, hh, 0, 64), start=True, stop=False)
                    S.mm(o_, hp(ktok, hh, hh * 64, (hh + 1) * 64), hp(vtok, hh, hh * 64, (hh + 1) * 64),
                         start=False, stop=True)
                S.tt(St, St, pS[:, 128:192], ALU.add)
                S.ts(St, St, Ep[:, b_ * 64 + 63:b_ * 64 + 64], ALU.mult)
            S.mm(PS[3].all(), bones_t.all(), ofull.all())
            S.stt(ofull.all(), PS[3].all(), -1.0 / 64, ofull.all(), ALU.mult, ALU.add)
            S.act(tmp.all(), ofull.all(), AF.Square)
            S.mm(PS[4].all(), bones_t.all(), tmp.all())
            S.act(tmp.all(), PS[4].all(), AF.Sqrt, bias=EPSLN, scale=1.0 / 64)
            S.recip(tmp.all(), tmp.all())
            S.stt(ofull.all(), ofull.all(), P("lnw%d" % l, g), tmp.all(), ALU.mult, ALU.mult)
            S.stt(ofull.all(), ofull.all(), P("lnb%d" % l, g), bonus.all(), ALU.add, ALU.add)
            S.tt(yT[:, 4 + g, :], ofull.all(), gate.all(), ALU.mult)

    yT = carve(YT_OFF, [128, NFC, TT], BF16)
    for ti in range(NT):
        for tb in range(4):
            xin = XIN[tb % 2]
            S.dma("sp", xin.all(), x_d[ti * TT + tb * 128:ti * TT + (tb + 1) * 128, :])
            for grp in range(4):
                pt = PS[6 + grp % 2]
                for j in range(4):
                    fc = grp * 4 + j
                    S.tr(pt[:, j * 128:(j + 1) * 128], xin[:, fc * 128:(fc + 1) * 128], ident)
                S.cp(xT[:, grp * 4:(grp + 1) * 4, tb * 128:(tb + 1) * 128],
                     pt.all().m(lambda ap: ap.rearrange("p (a b) -> p a b", a=4)), eng=("act" if grp % 2 else "dve"))
        for l in range(DEPTH):
            ffn(l, 0)
            mixer(l, yT)
            ffn(l, 1)
        rstd_of([xT[:, fc, :] for fc in range(NFC)], D, EPS6, ones_t.all())
        for fc in range(NFC):
            S.stt(xT[:, fc, :], xT[:, fc, :], P("fnorm", fc), RSTD.all(), ALU.mult, ALU.mult)
        for tb in range(4):
            xo = XIN[tb % 2]
            for grp in range(4):
                pt = PS[6 + grp % 2]
                for j in range(4):
                    fc = grp * 4 + j
                    S.tr(pt[:, j * 128:(j + 1) * 128], xT[:, fc, tb * 128:(tb + 1) * 128], ident)
                S.cp(xo[:, grp * 512:(grp + 1) * 512], pt.all(), eng=("act" if grp % 2 else "dve"))
            S.dma("sp", out_d[ti * TT + tb * 128:ti * TT + (tb + 1) * 128, :], xo.all())

    block = es.enter_context(nc.Block())
    S.emit(block)
    es.close()
    print("instr counts:", {e: len(S.ins[e]) for e in S.ENG})
    return nc


_CACHE = {}


def make_in_maps(inp, n_cores, depth=2):
    pv, ps = _pack(inp, depth)
    cst = _consts()
    f = lambda a: np.ascontiguousarray(np.asarray(a, np.float32))
    shared = {
        "pv": pv, "ps": ps, "consts": cst,
        "mod_w": f(inp["mod_w"]), "ffn_w1": f(inp["ffn_w1"]), "ffn_w3": f(inp["ffn_w3"]), "ffn_w2": f(inp["ffn_w2"]),
        "w_in": f(inp["w_in"]), "w_out": f(inp["w_out"]), "rw_w2": f(inp["rw_w2"]), "rw_a2": f(inp["rw_a2"]),
        "rw_g2": f(inp["rw_g2"]), "lru_wa": f(inp["lru_wa"]), "lru_wx": f(inp["lru_wx"]),
    }
    maps = []
    for b in range(n_cores):
        m = dict(shared)
        m["x"] = f(inp["x"][b])
        m["cT"] = _fm(inp["c"][b])
        maps.append(m)
    return maps


CORES_PER_LAUNCH = 2


def kernel(**inputs):
    x = np.asarray(inputs["x"])
    B, T, _ = x.shape
    NT = T // TT
    key = (NT,)
    if key not in _CACHE:
        _CACHE[key] = build(NT)
    nc = _CACHE[key]
    maps = make_in_maps(inputs, B)
    outs = []
    g = CORES_PER_LAUNCH
    for b0 in range(0, B, g):
        res = run_bass_kernel_spmd(nc, maps[b0:b0 + g], core_ids=list(range(len(maps[b0:b0 + g]))))
        outs.extend(np.asarray(r["out"], np.float32) for r in res.results)
    return np.stack(outs, axis=0)
```
